# Optimizing a Trainium2 kernel written in Bass

```python
import math
import jax, jax.numpy as jnp
from jax import lax
import numpy as np

D_MODEL = 2048
BATCH = 4
SEQ = 4096
DEPTH = 2

CHUNK = 64
D_MIX = D_MODEL
SSM_DIM = D_MIX // 4
SSM_GROUP_CH = 16
SSM_GROUPS = SSM_DIM // SSM_GROUP_CH
SSM_STATE = 64
CONV_DIM = D_MIX // 4
CONV_WIDTH = 3
ATTN_DIM = D_MIX - SSM_DIM - CONV_DIM
HEAD_DIM = 128
N_HEADS = ATTN_DIM // HEAD_DIM
IDX_HEADS = 16
IDX_DIM = 64
TOPK_MAX = 256
Q_BLOCK = CHUNK
D_FF = 4 * D_MODEL
ROPE_THETA = 10000.0
ALPHA = (2.0 * DEPTH) ** 0.25
BETA = (8.0 * DEPTH) ** -0.25
LN_EPS = 1e-5
RMS_EPS = 1e-6
DT_MIN = 1e-3
DT_MAX = 1e-1
IN_SPLITS = (SSM_DIM,
             CONV_DIM, CONV_DIM, CONV_DIM,
             ATTN_DIM, ATTN_DIM, ATTN_DIM,
             IDX_HEADS * IDX_DIM,
             IDX_DIM,
             IDX_HEADS)
D_IN = sum(IN_SPLITS)

kernel_name = "hybrid_s5_shortconv_dsa_deepnorm_adaln"


def layer_norm(x, g, b):
    xf = x.astype(jnp.float32)
    mu = jnp.mean(xf, axis=-1, keepdims=True)
    var = jnp.mean(jnp.square(xf - mu), axis=-1, keepdims=True)
    return ((xf - mu) * lax.rsqrt(var + LN_EPS) * g.astype(jnp.float32)
            + b.astype(jnp.float32)).astype(x.dtype)


def rms_norm(x, g):
    xf = x.astype(jnp.float32)
    y = xf * lax.rsqrt(jnp.mean(jnp.square(xf), axis=-1, keepdims=True) + RMS_EPS)
    return (y * g.astype(jnp.float32)).astype(x.dtype)


def rope_tables(seq, dim):
    inv = 1.0 / (ROPE_THETA ** (jnp.arange(0, dim, 2, dtype=jnp.float32) / dim))
    ang = jnp.arange(seq, dtype=jnp.float32)[:, None] * inv[None, :]
    return jnp.cos(ang), jnp.sin(ang)


def apply_rope(x, cos, sin):
    shp = (cos.shape[0],) + (1,) * (x.ndim - 3) + (cos.shape[1],)
    cos = cos.reshape(shp).astype(x.dtype)
    sin = sin.reshape(shp).astype(x.dtype)
    x1, x2 = jnp.split(x, 2, axis=-1)
    return jnp.concatenate([x1 * cos - x2 * sin, x2 * cos + x1 * sin], axis=-1)


def s5_mixer(u, lam_re, lam_im, log_dt, b_re, b_im, c_re, c_im, d_skip, w_glu, b_glu):
    bsz, seq, _ = u.shape
    f32 = jnp.float32
    uf = u.astype(f32).reshape(bsz, seq, SSM_GROUPS, SSM_GROUP_CH)
    lr, li = lam_re.astype(f32), lam_im.astype(f32)
    dt = jnp.exp(log_dt.astype(f32))[:, None]
    mag = jnp.exp(lr * dt)
    ang = li * dt
    lb_re, lb_im = mag * jnp.cos(ang), mag * jnp.sin(ang)
    den = lr * lr + li * li
    n_re, n_im = lb_re - 1.0, lb_im
    f_re = (n_re * lr + n_im * li) / den
    f_im = (n_im * lr - n_re * li) / den
    br, bi = b_re.astype(f32), b_im.astype(f32)
    bb_re = f_re[..., None] * br - f_im[..., None] * bi
    bb_im = f_re[..., None] * bi + f_im[..., None] * br
    bu_re = jnp.einsum('bsgh,gph->bsgp', uf, bb_re)
    bu_im = jnp.einsum('bsgh,gph->bsgp', uf, bb_im)
    a_re = jnp.broadcast_to(lb_re, bu_re.shape)
    a_im = jnp.broadcast_to(lb_im, bu_im.shape)

    def combine(left, right):
        a1r, a1i, b1r, b1i = left
        a2r, a2i, b2r, b2i = right
        return (a2r * a1r - a2i * a1i,
                a2r * a1i + a2i * a1r,
                a2r * b1r - a2i * b1i + b2r,
                a2r * b1i + a2i * b1r + b2i)

    _, _, xr, xi = lax.associative_scan(combine, (a_re, a_im, bu_re, bu_im), axis=1)
    y = (jnp.einsum('bsgp,ghp->bsgh', xr, c_re.astype(f32))
         - jnp.einsum('bsgp,ghp->bsgh', xi, c_im.astype(f32)))
    y = y.reshape(bsz, seq, SSM_DIM) + d_skip.astype(f32) * uf.reshape(bsz, seq, SSM_DIM)
    y = y.astype(u.dtype)
    g = jax.nn.gelu(y)
    return g * jax.nn.sigmoid(g @ w_glu + b_glu)


def short_conv_mixer(h, gate_b, gate_c, conv_w):
    z = gate_c * h
    z = lax.conv_general_dilated(z, conv_w[:, None, :].astype(z.dtype), window_strides=(1,),
                                 padding=[(CONV_WIDTH - 1, 0)],
                                 dimension_numbers=('NWC', 'WIO', 'NWC'),
                                 feature_group_count=CONV_DIM)
    return gate_b * z


def sparse_attention(q, k, v, qi, ki, wi):
    bsz, seq = q.shape[0], q.shape[1]
    topk = min(TOPK_MAX, seq // 4)
    nqb = seq // Q_BLOCK
    key_chunk = jnp.arange(seq) // CHUNK

    def to_blocks(a):
        a = a.reshape((bsz, nqb, Q_BLOCK) + a.shape[2:])
        return jnp.moveaxis(a, 1, 0)

    def block(args):
        qb, qib, wib, j = args
        q_chunk = (j * Q_BLOCK + jnp.arange(Q_BLOCK)) // CHUNK
        logits = jnp.einsum('bqhd,bsd->bqsh', qib, ki) * (IDX_DIM ** -0.5)
        score = jnp.einsum('bqsh,bqh->bqs', jax.nn.relu(logits), wib) * (IDX_HEADS ** -0.5)
        adm = key_chunk[None, :] <= q_chunk[:, None]
        score = jnp.where(adm[None], score.astype(jnp.float32), -jnp.inf)
        _, idx = lax.top_k(score, topk)
        valid = (idx // CHUNK) <= q_chunk[None, :, None]
        kg = jax.vmap(lambda kk, ii: kk[ii])(k, idx)
        vg = jax.vmap(lambda vv, ii: vv[ii])(v, idx)
        s = jnp.einsum('bqhd,bqkhd->bhqk', qb, kg).astype(jnp.float32) * (HEAD_DIM ** -0.5)
        s = jnp.where(valid[:, None], s, -jnp.inf)
        p = jax.nn.softmax(s, axis=-1).astype(vg.dtype)
        return jnp.einsum('bhqk,bqkhd->bqhd', p, vg)

    out = lax.map(block, (to_blocks(q), to_blocks(qi), to_blocks(wi), jnp.arange(nqb)))
    out = jnp.moveaxis(out, 0, 1).reshape(bsz, seq, N_HEADS * HEAD_DIM)
    return out


def token_mixer(h, w_in, lam_re, lam_im, log_dt, b_re, b_im, c_re, c_im, d_skip,
                w_glu, b_glu, conv_w, gnorm_g, w_o, cos_a, sin_a, cos_i, sin_i):
    bsz, seq, _ = h.shape
    proj = h @ w_in
    offsets = [int(o) for o in np.cumsum(IN_SPLITS)[:-1]]
    u, ch, cb, cc, q, k, v, qi, ki, wi = jnp.split(proj, offsets, axis=-1)
    y_ssm = s5_mixer(u, lam_re, lam_im, log_dt, b_re, b_im, c_re, c_im, d_skip, w_glu, b_glu)
    y_conv = short_conv_mixer(ch, cb, cc, conv_w)
    q = apply_rope(q.reshape(bsz, seq, N_HEADS, HEAD_DIM), cos_a, sin_a)
    k = apply_rope(k.reshape(bsz, seq, N_HEADS, HEAD_DIM), cos_a, sin_a)
    v = v.reshape(bsz, seq, N_HEADS, HEAD_DIM)
    qi = apply_rope(qi.reshape(bsz, seq, IDX_HEADS, IDX_DIM), cos_i, sin_i)
    ki = apply_rope(ki, cos_i, sin_i)
    y_attn = sparse_attention(q, k, v, qi, ki, wi)
    g_ssm, g_conv, g_attn = jnp.split(gnorm_g, [SSM_DIM, SSM_DIM + CONV_DIM])
    y = jnp.concatenate([rms_norm(y_ssm, g_ssm), rms_norm(y_conv, g_conv),
                         rms_norm(y_attn, g_attn)], axis=-1)
    return y @ w_o


def setup_inputs(seed: int = 0) -> dict:
    key = jax.random.key(seed)
    ks = jax.random.split(key, 32)
    f32 = jnp.float32
    L = DEPTH

    def nrm(k, shape, s):
        return jax.random.normal(k, shape, f32) * s

    n_idx = jnp.arange(SSM_STATE, dtype=f32)
    lam_re = -0.5 * (1.0 + nrm(ks[5], (L, SSM_GROUPS, SSM_STATE), 0.01))
    lam_im = (math.pi * n_idx * (1.0 + nrm(ks[6], (L, SSM_GROUPS, SSM_STATE), 0.01))
              + nrm(ks[7], (L, SSM_GROUPS, SSM_STATE), 0.01))
    log_dt = jax.random.uniform(ks[8], (L, SSM_GROUPS), f32,
                                math.log(DT_MIN), math.log(DT_MAX))
    return {
        "x": nrm(ks[0], (BATCH, SEQ, D_MODEL), 1.0),
        "c": nrm(ks[1], (BATCH, D_MODEL), 1.0),
        "w_ada": nrm(ks[2], (L, D_MODEL, 6 * D_MODEL), 0.5 * D_MODEL ** -0.5),
        "b_ada": nrm(ks[3], (L, 6 * D_MODEL), 0.01),
        "w_in": nrm(ks[4], (L, D_MODEL, D_IN), D_MODEL ** -0.5),
        "lam_re": lam_re,
        "lam_im": lam_im,
        "log_dt": log_dt,
        "ssm_b_re": nrm(ks[9], (L, SSM_GROUPS, SSM_STATE, SSM_GROUP_CH), (2 * SSM_GROUP_CH) ** -0.5),
        "ssm_b_im": nrm(ks[10], (L, SSM_GROUPS, SSM_STATE, SSM_GROUP_CH), (2 * SSM_GROUP_CH) ** -0.5),
        "ssm_c_re": nrm(ks[11], (L, SSM_GROUPS, SSM_GROUP_CH, SSM_STATE), (2 * SSM_STATE) ** -0.5),
        "ssm_c_im": nrm(ks[12], (L, SSM_GROUPS, SSM_GROUP_CH, SSM_STATE), (2 * SSM_STATE) ** -0.5),
        "ssm_d": nrm(ks[13], (L, SSM_DIM), 1.0),
        "w_glu": nrm(ks[14], (L, SSM_DIM, SSM_DIM), SSM_DIM ** -0.5),
        "b_glu": nrm(ks[15], (L, SSM_DIM), 0.01),
        "conv_w": nrm(ks[16], (L, CONV_WIDTH, CONV_DIM), CONV_WIDTH ** -0.5),
        "gnorm_g": 1.0 + nrm(ks[17], (L, D_MIX), 0.01),
        "w_o": nrm(ks[18], (L, D_MIX, D_MODEL), BETA * D_MIX ** -0.5),
        "ln1_g": 1.0 + nrm(ks[19], (L, D_MODEL), 0.01),
        "ln1_b": nrm(ks[20], (L, D_MODEL), 0.01),
        "w_ff1": nrm(ks[21], (L, D_MODEL, D_FF), D_MODEL ** -0.5),
        "w_ff2": nrm(ks[22], (L, D_FF, D_MODEL), BETA * D_FF ** -0.5),
        "ln2_g": 1.0 + nrm(ks[23], (L, D_MODEL), 0.01),
        "ln2_b": nrm(ks[24], (L, D_MODEL), 0.01),
    }


def reference(x, c, w_ada, b_ada, w_in, lam_re, lam_im, log_dt, ssm_b_re, ssm_b_im,
              ssm_c_re, ssm_c_im, ssm_d, w_glu, b_glu, conv_w, gnorm_g, w_o,
              ln1_g, ln1_b, w_ff1, w_ff2, ln2_g, ln2_b):
    seq = x.shape[1]
    cos_a, sin_a = rope_tables(seq, HEAD_DIM)
    cos_i, sin_i = rope_tables(seq, IDX_DIM)
    for l in range(DEPTH):
        mod = c @ w_ada[l] + b_ada[l]
        sh1, sc1, g1, sh2, sc2, g2 = [m[:, None, :] for m in jnp.split(mod, 6, axis=-1)]
        h = x * (1.0 + sc1) + sh1
        mix = token_mixer(h, w_in[l], lam_re[l], lam_im[l], log_dt[l], ssm_b_re[l], ssm_b_im[l],
                          ssm_c_re[l], ssm_c_im[l], ssm_d[l], w_glu[l], b_glu[l], conv_w[l],
                          gnorm_g[l], w_o[l], cos_a, sin_a, cos_i, sin_i)
        x = layer_norm(ALPHA * x + g1 * mix, ln1_g[l], ln1_b[l])
        h = x * (1.0 + sc2) + sh2
        ff = jnp.square(jax.nn.relu(h @ w_ff1[l])) @ w_ff2[l]
        x = layer_norm(ALPHA * x + g2 * ff, ln2_g[l], ln2_b[l])
    return x
```

```python
import numpy as np
from contextlib import ExitStack
import ml_dtypes
import concourse.bass as bass
import concourse.mybir as mybir
from concourse.bass_utils import run_bass_kernel_spmd

F32 = mybir.dt.float32
BF16 = mybir.dt.bfloat16
ALU = mybir.AluOpType
AF = mybir.ActivationFunctionType
AX = mybir.AxisListType

D = 2048
KC = 16
T = 2048
NTB = 4
TB = 512
DEPTH = 2
D_IN = 6224
D_FF = 8192
ALPHA = (2.0 * DEPTH) ** 0.25
LN_EPS = 1e-5
RMS_EPS = 1e-6
NEG = -30000.0
TOPK = 256
NIT = 22
SCH = 256
NSCH = T // SCH


class Prog:
    def __init__(self):
        self.nc = bass.Bass("TRN2", target_bir_lowering=False)
        nc = self.nc
        self.E = dict(pe=nc.tensor, act=nc.scalar, dve=nc.vector, pool=nc.gpsimd, sp=nc.sync)
        self.psem = {n: nc.alloc_semaphore("pg_" + n) for n in self.E}
        self.cnt = {n: 0 for n in self.E}
        self.seen = {n: {} for n in self.E}
        self.dsem = {}
        self.dram = {}

    def op(self, eng, ins):
        ins.then_inc(self.psem[eng], 1)
        self.cnt[eng] += 1
        return ("p", eng, self.cnt[eng])

    def wait(self, eng, *toks):
        for t in toks:
            if t is None:
                continue
            if isinstance(t, (list, tuple)) and (len(t) == 0 or not isinstance(t[0], str)):
                self.wait(eng, *t)
                continue
            kind, k, v = t
            if self.seen[eng].get((kind, k), 0) >= v:
                continue
            sem = self.psem[k] if kind == "p" else self.dsem[k][0]
            self.E[eng].wait_ge(sem, v)
            self.seen[eng][(kind, k)] = v

    def I(self, eng, deps, fn):
        self.wait(eng, deps)
        return self.op(eng, fn(self.E[eng]))

    def dma(self, eng, key, out, in_, deps=()):
        if key not in self.dsem:
            self.dsem[key] = [self.nc.alloc_semaphore("d_" + key), 0]
        self.wait(eng, deps)
        d = self.dsem[key]
        self.E[eng].dma_start(out=out, in_=in_).then_inc(d[0], 16)
        d[1] += 16
        return ("d", key, d[1])

    def phase_end(self):
        nc = self.nc
        for key, (h, c) in self.dsem.items():
            if c:
                self.wait("sp", ("d", key, c))
        nc.all_engine_barrier()
        for n in self.E:
            nc.gpsimd.sem_clear(self.psem[n])
        for key, (h, c) in self.dsem.items():
            nc.gpsimd.sem_clear(h)
            self.dsem[key][1] = 0
        nc.all_engine_barrier()
        self.cnt = {n: 0 for n in self.E}
        self.seen = {n: {} for n in self.E}
        if getattr(self, "C", None) is not None:
            self.C["tok"] = None

    def dt(self, name, shape, dtype, kind):
        if name not in self.dram:
            self.dram[name] = (self.nc.dram_tensor(name, list(shape), dtype, kind=kind).ap(), kind, list(shape), dtype)
        return self.dram[name][0]


def np_dt(dtype):
    return np.float32 if dtype == F32 else ml_dtypes.bfloat16


def make_consts(K):
    nc = K.nc
    c = {}
    c["idb"] = nc.alloc_sbuf_tensor("c_idb", [128, 128], BF16)
    c["idf"] = nc.alloc_sbuf_tensor("c_idf", [128, 128], F32)
    c["onesb"] = nc.alloc_sbuf_tensor("c_onesb", [128, 128], BF16)
    c["onesf"] = nc.alloc_sbuf_tensor("c_onesf", [128, 128], F32)
    toks = []
    for nm in ("idb", "idf"):
        K.I("pool", (), lambda e: e.memset(c[nm][:], 1.0))
        toks.append(K.I("pool", (), lambda e: e.affine_select(out=c[nm][:], in_=c[nm][:], pattern=[[-1, 128]],
                                                              compare_op=ALU.is_equal, fill=0.0, base=0,
                                                              channel_multiplier=1)))
    toks.append(K.I("pool", (), lambda e: e.memset(c["onesb"][:], 1.0)))
    toks.append(K.I("pool", (), lambda e: e.memset(c["onesf"][:], 1.0)))
    c["eps_rms"] = nc.alloc_sbuf_tensor("c_epsr", [128, 1], F32)
    c["eps_ln"] = nc.alloc_sbuf_tensor("c_epsl", [128, 1], F32)
    c["cmask"] = nc.alloc_sbuf_tensor("c_cmask", [128, 128], BF16)
    K.I("pool", (), lambda e: e.memset(c["eps_rms"][:], RMS_EPS))
    K.I("pool", (), lambda e: e.memset(c["eps_ln"][:], LN_EPS / (ALPHA * ALPHA)))
    K.I("pool", (), lambda e: e.memset(c["cmask"][:], 0.0))
    toks.append(K.I("pool", (), lambda e: e.memset(c["cmask"][0:64, 64:128], NEG)))
    c["tok"] = toks[-1]
    K.C = c
    return c


def stage_A(K, C, l, io):
    nc = K.nc
    xT = io["xT"]
    w_in = io["w_in"]
    modT = io["modT"]
    with nc.sbuf_tensor("a0_c", [128, KC], F32) as c_sb, \
         nc.sbuf_tensor("a0_b", [128, 96], F32) as b_sb, \
         nc.sbuf_tensor("a0_m", [128, 96], F32) as m_sb, \
         nc.sbuf_tensor("a0_w", [128, 2, KC, 384], F32) as w_sb, \
         nc.psum_tensor("a0_ps", [128, 96], F32) as ps:
        t_c = K.dma("sp", "a0c", c_sb[:], io["cvec"][:, :])
        t_b = K.dma("sp", "a0b", b_sb[:], io["b_ada"][:, :])
        free = [None, None]
        last_mm = None
        for blk in range(32):
            s = blk % 2
            t_w = K.dma("sp" if blk % 2 == 0 else "act", "a0w%d" % s, w_sb[:, s, :, :],
                        io["w_ada"][:, :, blk * 384:(blk + 1) * 384], deps=[free[s]])
            for j in range(3):
                jb = blk * 3 + j
                for kc in range(KC):
                    last_mm = K.I("pe", [t_w, t_c], lambda e: e.matmul(
                        ps[:, jb:jb + 1], lhsT=w_sb[:, s, kc, j * 128:(j + 1) * 128], rhs=c_sb[:, kc:kc + 1],
                        start=(kc == 0), stop=(kc == KC - 1)))
            free[s] = last_mm
        t_m = K.I("dve", [last_mm, t_b], lambda e: e.tensor_tensor(out=m_sb[:], in0=ps[:], in1=b_sb[:], op=ALU.add))
        K.dma("sp", "a0o", modT[:, :], m_sb[:], deps=[t_m])
    K.phase_end()

    with ExitStack() as es:
        hT = es.enter_context(nc.sbuf_tensor("a_hT", [128, KC, T], BF16))
        mod = es.enter_context(nc.sbuf_tensor("a_mod", [128, 96], F32))
        sc1p = es.enter_context(nc.sbuf_tensor("a_sc1p", [128, KC], F32))
        xin = es.enter_context(nc.sbuf_tensor("a_xin", [128, 4, TB], F32))
        wsb = es.enter_context(nc.sbuf_tensor("a_w", [128, 4, KC, 128], BF16))
        cosA = es.enter_context(nc.sbuf_tensor("a_cos", [128, T], F32))
        sinA = es.enter_context(nc.sbuf_tensor("a_sin", [128, T], F32))
        cosI = es.enter_context(nc.sbuf_tensor("a_cosi", [128, T], F32))
        sinI = es.enter_context(nc.sbuf_tensor("a_sini", [128, T], F32))
        cw = es.enter_context(nc.sbuf_tensor("a_cw", [128, 4, 3], F32))
        xs = es.enter_context(nc.sbuf_tensor("a_xs", [128, 2, TB], F32))
        xw = es.enter_context(nc.sbuf_tensor("a_xw", [128, 2, TB], F32))
        t1b = es.enter_context(nc.sbuf_tensor("a_t1", [128, 2, TB], F32))
        ob = es.enter_context(nc.sbuf_tensor("a_ob", [128, 2, TB], BF16))
        of = es.enter_context(nc.sbuf_tensor("a_of", [128, 2, TB], F32))
        zb = es.enter_context(nc.sbuf_tensor("a_z", [128, 2, TB + 2], F32))
        vt = es.enter_context(nc.sbuf_tensor("a_vt", [128, 2, 4, 128], BF16))
        wt = es.enter_context(nc.sbuf_tensor("a_wt", [128, 2, 4, 16], F32))
        ps = es.enter_context(nc.psum_tensor("a_ps", [128, 6, TB], F32))
        pt = es.enter_context(nc.psum_tensor("a_pt", [128, 2, 4, 128], BF16))
        pw = es.enter_context(nc.psum_tensor("a_pw", [128, 4, 16], F32))
        t_mod = K.dma("sp", "a1m", mod[:], modT[:, :])
        t_tab = K.dma("sp", "a1t", cosA[:], io["cosA"][:, :])
        t_tab = K.dma("sp", "a1t", sinA[:], io["sinA"][:, :])
        t_tab = K.dma("act", "a1t", cosI[:], io["cosI"][:, :])
        t_tab = K.dma("act", "a1t", sinI[:], io["sinI"][:, :])
        t_tab = K.dma("act", "a1t", cw[:], io["conv_w"][:, :, :])
        t_sc = K.I("dve", [t_mod], lambda e: e.tensor_scalar(out=sc1p[:], in0=mod[:, 16:32], scalar1=1.0, scalar2=None, op0=ALU.add))
        free = [None] * 4
        t_h = None
        n = 0
        for tb in range(NTB):
            for kc in range(KC):
                s = n % 4
                t_x = K.dma("sp" if n % 2 == 0 else "act", "a1x%d" % s, xin[:, s, :],
                            xT[kc * 128:(kc + 1) * 128, tb * TB:(tb + 1) * TB], deps=[free[s]])
                t_h = K.I("act", [t_x, t_sc], lambda e: e.activation(
                    out=hT[:, kc, tb * TB:(tb + 1) * TB], in_=xin[:, s, :], func=AF.Identity,
                    bias=mod[:, kc:kc + 1], scale=sc1p[:, kc:kc + 1]))
                free[s] = t_h
                n += 1
        t_hall = t_h

        wfree = [None] * 4
        wcnt = [0]
        psfree = [None] * 6

        def load_w(cb, ncols=128):
            s = wcnt[0] % 4
            wcnt[0] += 1
            t = K.dma("pool", "a2w%d" % s, wsb[:, s, :, 0:ncols], w_in[:, :, cb * 128:cb * 128 + ncols], deps=[wfree[s]])
            return s, t

        def mm_block(s, t_w, tb, bank, ncols=128):
            last = None
            for kc in range(KC):
                last = K.I("pe", [t_w, t_hall, psfree[bank]], lambda e: e.matmul(
                    ps[0:ncols, bank, :], lhsT=wsb[:, s, kc, 0:ncols], rhs=hT[:, kc, tb * TB:(tb + 1) * TB],
                    start=(kc == 0), stop=(kc == KC - 1)))
            wfree[s] = last
            return last

        bankc = [0]

        def nb():
            b = bankc[0] % 6
            bankc[0] += 1
            return b

        ebuf = [0]
        efree = [None, None]

        def stg():
            i = ebuf[0] % 2
            ebuf[0] += 1
            return i

        for cb in range(0, 4):
            s, t_w = load_w(cb)
            for tb in range(NTB):
                b = nb()
                t_mm = mm_block(s, t_w, tb, b)
                i = stg()
                t_c = K.I("act", [t_mm, efree[i]], lambda e: e.activation(out=of[:, i, :], in_=ps[:, b, :], func=AF.Copy))
                psfree[b] = t_c
                efree[i] = K.dma("sp", "a2o%d" % i, io["uT"][cb * 128:(cb + 1) * 128, tb * TB:(tb + 1) * TB], of[:, i, :], deps=[t_c])

        for j in range(4):
            s1, tw1 = load_w(4 + j)
            s2, tw2 = load_w(8 + j)
            s3, tw3 = load_w(12 + j)
            t_zprev = None
            for tb in range(NTB):
                b1, b2, b3 = nb(), nb(), nb()
                m1 = mm_block(s1, tw1, tb, b1)
                m2 = mm_block(s2, tw2, tb, b2)
                m3 = mm_block(s3, tw3, tb, b3)
                i = stg()
                zi = tb % 2
                t_ch = K.I("act", [m1, efree[i]], lambda e: e.activation(out=xs[:, i, :], in_=ps[:, b1, :], func=AF.Copy))
                psfree[b1] = t_ch
                if tb == 0:
                    t_hl = K.I("dve", [t_zprev, efree[i]], lambda e: e.memset(zb[:, zi, 0:2], 0.0))
                else:
                    t_hl = K.I("dve", [t_zprev, efree[i]], lambda e: e.tensor_copy(out=zb[:, zi, 0:2], in_=zb[:, 1 - zi, TB:TB + 2]))
                t_z = K.I("dve", [m3, t_ch, t_hl], lambda e: e.tensor_tensor(out=zb[:, zi, 2:TB + 2], in0=ps[:, b3, :], in1=xs[:, i, :], op=ALU.mult))
                psfree[b3] = t_z
                t_a = K.I("dve", [t_z, t_tab], lambda e: e.tensor_scalar(out=t1b[:, i, :], in0=zb[:, zi, 2:TB + 2], scalar1=cw[:, j, 2:3], scalar2=None, op0=ALU.mult))
                t_a = K.I("dve", [t_a], lambda e: e.scalar_tensor_tensor(out=t1b[:, i, :], in0=zb[:, zi, 1:TB + 1], scalar=cw[:, j, 1:2], in1=t1b[:, i, :], op0=ALU.mult, op1=ALU.add))
                t_a = K.I("dve", [t_a], lambda e: e.scalar_tensor_tensor(out=t1b[:, i, :], in0=zb[:, zi, 0:TB], scalar=cw[:, j, 0:1], in1=t1b[:, i, :], op0=ALU.mult, op1=ALU.add))
                t_y = K.I("dve", [t_a, m2], lambda e: e.tensor_tensor(out=of[:, i, :], in0=ps[:, b2, :], in1=t1b[:, i, :], op=ALU.mult))
                t_zprev = t_y
                t_o = K.dma("sp", "a2o%d" % i, io["ycT"][j * 128:(j + 1) * 128, tb * TB:(tb + 1) * TB], of[:, i, :], deps=[t_y])
                if tb == 0:
                    t_cf = K.I("act", [m2, t_ch], lambda e: e.activation(out=xw[:, i, 0:2], in_=ps[:, b2, 0:2], func=AF.Copy))
                    t_o = K.dma("sp", "a2o%d" % i, io["cbf"][j * 128:(j + 1) * 128, :], xw[:, i, 0:2], deps=[t_cf])
                    psfree[b2] = [t_y, t_cf]
                else:
                    psfree[b2] = t_y
                if tb == NTB - 1:
                    t_o = K.dma("sp", "a2o%d" % i, io["zlast"][j * 128:(j + 1) * 128, :], zb[:, zi, TB:TB + 2], deps=[t_z])
                efree[i] = t_o

        def rope(b, i, m, hs, cosT, sinT, tb, scale, nrows=128):
            t_a = K.I("act", [m, efree[i]], lambda e: e.activation(out=xs[0:nrows, i, :], in_=ps[0:nrows, b, :], func=AF.Copy, scale=scale))
            t_b = None
            for g in range(0, nrows, 2 * hs):
                K.I("act", [m, efree[i]], lambda e: e.activation(out=xw[g:g + hs, i, :], in_=ps[g + hs:g + 2 * hs, b, :], func=AF.Copy, scale=scale))
                t_b = K.I("act", [m, efree[i]], lambda e: e.activation(out=xw[g + hs:g + 2 * hs, i, :], in_=ps[g:g + hs, b, :], func=AF.Copy, scale=scale))
            psfree[b] = t_b
            t_c = K.I("dve", [t_a, t_tab], lambda e: e.tensor_tensor(out=t1b[0:nrows, i, :], in0=xs[0:nrows, i, :], in1=cosT[0:nrows, tb * TB:(tb + 1) * TB], op=ALU.mult))
            t_d = K.I("pool", [t_b, t_tab], lambda e: e.tensor_tensor(out=xw[0:nrows, i, :], in0=xw[0:nrows, i, :], in1=sinT[0:nrows, tb * TB:(tb + 1) * TB], op=ALU.mult))
            t_e = K.I("dve", [t_c, t_d], lambda e: e.tensor_tensor(out=ob[0:nrows, i, :], in0=t1b[0:nrows, i, :], in1=xw[0:nrows, i, :], op=ALU.add))
            return t_e

        for cb in range(16, 32):
            s, t_w = load_w(cb)
            isq = cb < 24
            dst = io["qT"] if isq else io["kT"]
            r0 = (cb - (16 if isq else 24)) * 128
            for tb in range(NTB):
                b = nb()
                m = mm_block(s, t_w, tb, b)
                i = stg()
                t_e = rope(b, i, m, 64, cosA, sinA, tb, (128.0 ** -0.5) if isq else 1.0)
                efree[i] = K.dma("sp", "a2o%d" % i, dst[r0:r0 + 128, tb * TB:(tb + 1) * TB], ob[:, i, :], deps=[t_e])

        vfree = [None, None]
        vc = 0
        for cb in range(32, 40):
            s, t_w = load_w(cb)
            hh = cb - 32
            for tb in range(NTB):
                b = nb()
                m = mm_block(s, t_w, tb, b)
                i = stg()
                t_c = K.I("act", [m, efree[i]], lambda e: e.activation(out=ob[:, i, :], in_=ps[:, b, :], func=AF.Copy))
                psfree[b] = t_c
                vi = vc % 2
                vc += 1
                t_t = None
                for q4 in range(4):
                    t_t = K.I("pe", [t_c, vfree[0], vfree[1], C["tok"]], lambda e: e.transpose(pt[:, vi, q4, :], ob[:, i, q4 * 128:(q4 + 1) * 128], C["idb"][:]))
                efree[i] = t_t
                t_v = K.I("dve", [t_t], lambda e: e.tensor_copy(out=vt[:, vi, :, :], in_=pt[:, vi, :, :]))
                t_o = K.dma("sp", "a2v%d" % vi, io["vtok"][tb * TB:(tb + 1) * TB, hh * 128:(hh + 1) * 128].rearrange("(q p) d -> p q d", p=128),
                            vt[:, vi, :, :], deps=[t_v])
                vfree[vi] = [t_v, t_o]

        for cb in range(40, 48):
            s, t_w = load_w(cb)
            r0 = (cb - 40) * 128
            for tb in range(NTB):
                b = nb()
                m = mm_block(s, t_w, tb, b)
                i = stg()
                t_e = rope(b, i, m, 32, cosI, sinI, tb, 1.0)
                efree[i] = K.dma("sp", "a2o%d" % i, io["qiT"][r0:r0 + 128, tb * TB:(tb + 1) * TB], ob[:, i, :], deps=[t_e])

        s, t_w = load_w(48, 80)
        wfree_t = [None, None]
        for tb in range(NTB):
            b = nb()
            m = mm_block(s, t_w, tb, b, 80)
            i = stg()
            t_wi = K.I("act", [m, efree[i]], lambda e: e.activation(out=of[64:80, i, :], in_=ps[64:80, b, :], func=AF.Copy, scale=1.0 / 32.0))
            t_e = rope(b, i, m, 32, cosI, sinI, tb, 1.0, nrows=64)
            psfree[b] = [psfree[b], t_wi]
            t_o = K.dma("sp", "a2o%d" % i, io["kiT"][:, tb * TB:(tb + 1) * TB], ob[0:64, i, :], deps=[t_e])
            wi_i = tb % 2
            t_t = None
            for q4 in range(4):
                t_t = K.I("pe", [t_wi, wfree_t[wi_i], C["tok"]], lambda e: e.transpose(pw[:, q4, :], of[64:80, i, q4 * 128:(q4 + 1) * 128], C["idf"][64:80, 64:80]))
            t_c = K.I("dve", [t_t], lambda e: e.tensor_copy(out=wt[:, wi_i, :, :], in_=pw[:, :, :]))
            t_o2 = K.dma("sp", "a2w_o%d" % wi_i, io["witok"][tb * TB:(tb + 1) * TB, :].rearrange("(q p) d -> p q d", p=128), wt[:, wi_i, :, :], deps=[t_c])
            wfree_t[wi_i] = [t_c, t_o2]
            efree[i] = [t_o, t_t]
            wfree_t[1 - wi_i] = [wfree_t[1 - wi_i], t_c]
    K.phase_end()


def bc3(ap2, n):
    return ap2.unsqueeze(2).to_broadcast([ap2.shape[0], ap2.shape[1], n])


def stage_B1(K, C, io):
    nc = K.nc
    PI = float(np.pi)
    with ExitStack() as es:
        sb = lambda n, shp, d: es.enter_context(nc.sbuf_tensor(n, shp, d))
        lr = sb("s_lr", [128, 16], F32); li = sb("s_li", [128, 16], F32); ldt = sb("s_ldt", [128, 16], F32)
        br = sb("s_br", [128, 16, 16], F32); bi = sb("s_bi", [128, 16, 16], F32)
        cr = sb("s_cr", [128, 16, 16], F32); ci = sb("s_ci", [128, 16, 16], F32)
        dsk = sb("s_d", [128, 4], F32); bgl = sb("s_bg", [128, 4], F32); gn = sb("s_gn", [128, 16], F32)
        wgl = sb("s_wg", [128, 4, 512], BF16)
        tmp = sb("s_tmp", [128, 12, 16], F32)
        mag = sb("s_mag", [128, 16], F32); e1r = sb("s_e1r", [128, 16], F32); e1i = sb("s_e1i", [128, 16], F32)
        pwr = sb("s_pwr", [128, 16], F32); pwi = sb("s_pwi", [128, 16], F32)
        e256r = sb("s_e256r", [128, 16], F32); e256i = sb("s_e256i", [128, 16], F32); e256in = sb("s_e256in", [128, 16], F32)
        fre = sb("s_fre", [128, 16], F32); fim = sb("s_fim", [128, 16], F32)
        bbr = sb("s_bbr", [128, 16, 16], F32); bbi = sb("s_bbi", [128, 16, 16], F32); bt2 = sb("s_bt2", [128, 16, 16], F32)
        bpr = sb("s_bpr", [128, 16, 32], F32); bpi = sb("s_bpi", [128, 16, 32], F32)
        btr = sb("s_btr", [32, 16, 128], BF16); bti = sb("s_bti", [32, 16, 128], BF16)
        cpr = sb("s_cpr", [128, 16, 128], BF16); cpi = sb("s_cpi", [128, 16, 128], BF16)
        cE = sb("s_cE", [128, 16, SCH], F32); sE = sb("s_sE", [128, 16, SCH], F32); tE = sb("s_tE", [128, 16, 128], F32)
        zin = sb("s_zin", [128, 2, 2, 16], F32)
        ubf = sb("s_ubf", [32, 2, 16, SCH], BF16)
        uf = sb("s_uf", [128, 2, 4, SCH], F32)
        wk = sb("s_wk", [128, 2, 6, SCH], F32)
        zz = sb("s_zz", [128, 2, 2, SCH], F32)
        xb = sb("s_xb", [128, 16, 2, SCH], BF16)
        gg = sb("s_gg", [128, 4, SCH], F32); gb = sb("s_gb", [128, 4, SCH], BF16)
        ys = sb("s_ys", [128, 4, SCH], F32); sq = sb("s_sq", [128, 4, SCH], F32)
        rs = sb("s_rs", [128, SCH], F32); yo = sb("s_yo", [128, 2, 4, SCH], BF16)
        ptp = es.enter_context(nc.psum_tensor("s_ptp", [32, 2, 128], F32))
        pbu = es.enter_context(nc.psum_tensor("s_pbu", [128, 2, 2, SCH], F32))
        pyy = es.enter_context(nc.psum_tensor("s_pyy", [128, 2, SCH], F32))
        pgl = es.enter_context(nc.psum_tensor("s_pgl", [128, 2, SCH], F32))
        pss = es.enter_context(nc.psum_tensor("s_pss", [128, SCH], F32))

        tl = K.dma("sp", "b1p", lr[:], io["lam_re"][:, :]); K.dma("sp", "b1p", li[:], io["lam_im"][:, :])
        K.dma("sp", "b1p", ldt[:], io["log_dt"][:, :]); K.dma("sp", "b1p", br[:], io["ssm_b_re"][:, :, :])
        K.dma("sp", "b1p", bi[:], io["ssm_b_im"][:, :, :]); K.dma("act", "b1p", cr[:], io["ssm_c_re"][:, :, :])
        K.dma("act", "b1p", ci[:], io["ssm_c_im"][:, :, :]); K.dma("act", "b1p", dsk[:], io["ssm_d"][:, :])
        K.dma("act", "b1p", bgl[:], io["b_glu"][:, :])
        tl = K.dma("act", "b1p", gn[:], io["gnorm"][:, :])
        tw = K.dma("pool", "b1w", wgl[:], io["w_glu"][:, :, :])

        last = [tl]

        def V(fn, eng="dve", extra=()):
            last[0] = K.I(eng, [last[0], extra], fn)
            return last[0]
        T_ = lambda k: tmp[:, k, :]
        dt_, a_, ang, kac = T_(0), T_(1), T_(2), T_(3)
        V(lambda e: e.activation(out=dt_, in_=ldt[:], func=AF.Exp), "act")
        V(lambda e: e.tensor_tensor(out=a_, in0=lr[:], in1=dt_, op=ALU.mult))
        V(lambda e: e.activation(out=mag[:], in_=a_, func=AF.Exp), "act")
        V(lambda e: e.tensor_tensor(out=ang, in0=li[:], in1=dt_, op=ALU.mult))
        V(lambda e: e.memset(kac, 0.0))
        for j in range(8):
            V(lambda e: e.scalar_tensor_tensor(out=kac, in0=ang, scalar=(2 * j + 1) * PI, in1=kac, op0=ALU.is_gt, op1=ALU.add))
        angr, ang2, s2 = T_(4), T_(5), T_(6)
        V(lambda e: e.scalar_tensor_tensor(out=angr, in0=kac, scalar=-2.0 * PI, in1=ang, op0=ALU.mult, op1=ALU.add))
        V(lambda e: e.tensor_scalar(out=s2, in0=angr, scalar1=PI / 2, scalar2=-2.0 * PI, op0=ALU.is_gt, op1=ALU.mult))
        V(lambda e: e.scalar_tensor_tensor(out=ang2, in0=angr, scalar=PI / 2, in1=s2, op0=ALU.add, op1=ALU.add))
        V(lambda e: e.activation(out=e1i[:], in_=angr, func=AF.Sin), "act")
        V(lambda e: e.activation(out=e1r[:], in_=ang2, func=AF.Sin), "act")
        lbr, lbi, den, nre, t7, t8 = T_(7), T_(8), T_(9), T_(10), T_(11), T_(0)
        V(lambda e: e.tensor_tensor(out=lbr, in0=mag[:], in1=e1r[:], op=ALU.mult))
        V(lambda e: e.tensor_tensor(out=lbi, in0=mag[:], in1=e1i[:], op=ALU.mult))
        V(lambda e: e.tensor_tensor(out=den, in0=lr[:], in1=lr[:], op=ALU.mult))
        V(lambda e: e.tensor_tensor(out=t7, in0=li[:], in1=li[:], op=ALU.mult))
        V(lambda e: e.tensor_tensor(out=den, in0=den, in1=t7, op=ALU.add))
        V(lambda e: e.reciprocal(out=den, in_=den))
        V(lambda e: e.tensor_scalar(out=nre, in0=lbr, scalar1=-1.0, scalar2=None, op0=ALU.add))
        V(lambda e: e.tensor_tensor(out=t7, in0=nre, in1=lr[:], op=ALU.mult))
        V(lambda e: e.tensor_tensor(out=t8, in0=lbi, in1=li[:], op=ALU.mult))
        V(lambda e: e.tensor_tensor(out=t7, in0=t7, in1=t8, op=ALU.add))
        V(lambda e: e.tensor_tensor(out=fre[:], in0=t7, in1=den, op=ALU.mult))
        V(lambda e: e.tensor_tensor(out=t7, in0=lbi, in1=lr[:], op=ALU.mult))
        V(lambda e: e.tensor_tensor(out=t8, in0=nre, in1=li[:], op=ALU.mult))
        V(lambda e: e.tensor_tensor(out=t7, in0=t7, in1=t8, op=ALU.subtract))
        V(lambda e: e.tensor_tensor(out=fim[:], in0=t7, in1=den, op=ALU.mult))
        V(lambda e: e.tensor_tensor(out=bbr[:], in0=br[:], in1=bc3(fre[:], 16), op=ALU.mult))
        V(lambda e: e.tensor_tensor(out=bt2[:], in0=bi[:], in1=bc3(fim[:], 16), op=ALU.mult))
        V(lambda e: e.tensor_tensor(out=bbr[:], in0=bbr[:], in1=bt2[:], op=ALU.subtract))
        V(lambda e: e.tensor_tensor(out=bbi[:], in0=bi[:], in1=bc3(fre[:], 16), op=ALU.mult))
        V(lambda e: e.tensor_tensor(out=bt2[:], in0=br[:], in1=bc3(fim[:], 16), op=ALU.mult))
        V(lambda e: e.tensor_tensor(out=bbi[:], in0=bbi[:], in1=bt2[:], op=ALU.add))
        for (bp, bb) in ((bpr, bbr), (bpi, bbi)):
            V(lambda e: e.memset(bp[:], 0.0))
            V(lambda e: e.tensor_copy(out=bp[0:64, :, 0:16], in_=bb[0:64, :, :]))
            V(lambda e: e.tensor_copy(out=bp[64:128, :, 16:32], in_=bb[64:128, :, :]))
        t_bp = last[0]
        tfree = [None, None]
        t_bt = None
        n = 0
        for (bp, bt) in ((bpr, btr), (bpi, bti)):
            for s_ in range(16):
                i = 0
                n += 1
                t_t = K.I("pe", [t_bp, tfree[i], C["tok"]], lambda e: e.transpose(ptp[:, i, :], bp[:, s_, :], C["idf"][:]))
                t_bt = K.I("act", [t_t], lambda e: e.activation(out=bt[:, s_, :], in_=ptp[:, i, :], func=AF.Copy))
                tfree[i] = t_bt
        V(lambda e: e.memset(cpr[:], 0.0)); V(lambda e: e.memset(cpi[:], 0.0))
        for s_ in range(16):
            c0 = (s_ % 4) * 32
            V(lambda e: e.tensor_copy(out=cpr[0:64, s_, c0:c0 + 16], in_=cr[0:64, s_, :]))
            V(lambda e: e.tensor_copy(out=cpr[64:128, s_, c0 + 16:c0 + 32], in_=cr[64:128, s_, :]))
            V(lambda e: e.tensor_scalar(out=cpi[0:64, s_, c0:c0 + 16], in0=ci[0:64, s_, :], scalar1=-1.0, scalar2=None, op0=ALU.mult))
            V(lambda e: e.tensor_scalar(out=cpi[64:128, s_, c0 + 16:c0 + 32], in0=ci[64:128, s_, :], scalar1=-1.0, scalar2=None, op0=ALU.mult))
        V(lambda e: e.memset(cE[:, :, 0:1], 1.0)); V(lambda e: e.memset(sE[:, :, 0:1], 0.0))
        V(lambda e: e.tensor_copy(out=pwr[:], in_=e1r[:])); V(lambda e: e.tensor_copy(out=pwi[:], in_=e1i[:]))
        ln_ = 1
        while ln_ < SCH:
            V(lambda e: e.tensor_tensor(out=cE[:, :, ln_:2 * ln_], in0=cE[:, :, 0:ln_], in1=bc3(pwr[:], ln_), op=ALU.mult))
            V(lambda e: e.tensor_tensor(out=tE[:, :, 0:ln_], in0=sE[:, :, 0:ln_], in1=bc3(pwi[:], ln_), op=ALU.mult))
            V(lambda e: e.tensor_tensor(out=cE[:, :, ln_:2 * ln_], in0=cE[:, :, ln_:2 * ln_], in1=tE[:, :, 0:ln_], op=ALU.subtract))
            V(lambda e: e.tensor_tensor(out=sE[:, :, ln_:2 * ln_], in0=sE[:, :, 0:ln_], in1=bc3(pwr[:], ln_), op=ALU.mult))
            V(lambda e: e.tensor_tensor(out=tE[:, :, 0:ln_], in0=cE[:, :, 0:ln_], in1=bc3(pwi[:], ln_), op=ALU.mult))
            V(lambda e: e.tensor_tensor(out=sE[:, :, ln_:2 * ln_], in0=sE[:, :, ln_:2 * ln_], in1=tE[:, :, 0:ln_], op=ALU.add))
            V(lambda e: e.tensor_tensor(out=t7, in0=pwr[:], in1=pwr[:], op=ALU.mult))
            V(lambda e: e.tensor_tensor(out=t8, in0=pwi[:], in1=pwi[:], op=ALU.mult))
            V(lambda e: e.tensor_tensor(out=pwi[:], in0=pwr[:], in1=pwi[:], op=ALU.mult))
            V(lambda e: e.tensor_scalar(out=pwi[:], in0=pwi[:], scalar1=2.0, scalar2=None, op0=ALU.mult))
            V(lambda e: e.tensor_tensor(out=pwr[:], in0=t7, in1=t8, op=ALU.subtract))
            ln_ *= 2
        V(lambda e: e.tensor_copy(out=e256r[:], in_=pwr[:])); V(lambda e: e.tensor_copy(out=e256i[:], in_=pwi[:]))
        V(lambda e: e.tensor_scalar(out=e256in[:], in0=pwi[:], scalar1=-1.0, scalar2=None, op0=ALU.mult))
        V(lambda e: e.memset(zin[:], 0.0))
        t_prep = [last[0], t_bt]

        ufree = [None, None]
        uffree = [None, None]
        wkfree = [None, None]
        xbfree = None
        pbufree = [None, None]
        yofree = [None, None]
        glfree = None
        zin_tok = [[None] * 16, [None] * 16]
        zin_rd = [[None] * 16, [None] * 16]
        gl_tok = None
        for c in range(2 * NSCH):
            own = c >= NSCH
            src = io["uT"] if own else io["uT_prev"]
            c0 = (c % NSCH) * SCH
            ui = c % 2
            t_u = K.dma("pool", "b1u%d" % ui, ubf[:, ui, :, :], src[:, c0:c0 + SCH].rearrange("(s r) t -> r s t", r=32), deps=[ufree[ui]])
            par = c % 2
            t_x = []
            for s_ in range(16):
                k = s_ % 2
                mm1 = K.I("pe", [t_u, t_prep, pbufree[k]], lambda e: e.matmul(pbu[:, k, 0, :], lhsT=btr[:, s_, :], rhs=ubf[:, ui, s_, :], start=True, stop=True))
                mm2 = K.I("pe", [], lambda e: e.matmul(pbu[:, k, 1, :], lhsT=bti[:, s_, :], rhs=ubf[:, ui, s_, :], start=True, stop=True))
                cs, sn = cE[:, s_, :], sE[:, s_, :]
                w = lambda q: wk[:, k, q, :]
                a1 = K.I("dve", [mm2, wkfree[k], t_prep], lambda e: e.tensor_tensor(out=w(0), in0=pbu[:, k, 0, :], in1=cs, op=ALU.mult))
                a2 = K.I("dve", [], lambda e: e.tensor_tensor(out=w(1), in0=pbu[:, k, 1, :], in1=sn, op=ALU.mult))
                a3 = K.I("dve", [], lambda e: e.tensor_tensor(out=w(2), in0=pbu[:, k, 1, :], in1=cs, op=ALU.mult))
                a4 = K.I("dve", [], lambda e: e.tensor_tensor(out=w(3), in0=pbu[:, k, 0, :], in1=sn, op=ALU.mult))
                pbufree[k] = a4
                p1 = K.I("pool", [a2], lambda e: e.tensor_tensor(out=w(0), in0=w(0), in1=w(1), op=ALU.add))
                p2 = K.I("pool", [a4], lambda e: e.tensor_tensor(out=w(2), in0=w(2), in1=w(3), op=ALU.subtract))
                rho = mag[:, s_:s_ + 1].to_broadcast([128, SCH])
                z1 = K.I("dve", [p1, zin_tok[par][s_]], lambda e: e.tensor_tensor_scan(out=zz[:, k, 0, :], data0=rho, data1=w(0), initial=zin[:, par, 0, s_:s_ + 1], op0=ALU.mult, op1=ALU.add))
                z2 = K.I("dve", [p2], lambda e: e.tensor_tensor_scan(out=zz[:, k, 1, :], data0=rho, data1=w(2), initial=zin[:, par, 1, s_:s_ + 1], op0=ALU.mult, op1=ALU.add))
                zin_rd[par][s_] = z2
                if c < 2 * NSCH - 1:
                    np_ = 1 - par
                    q1 = K.I("dve", [z2, zin_rd[np_][s_]], lambda e: e.tensor_scalar(out=tmp[:, 0, s_:s_ + 1], in0=zz[:, k, 0, SCH - 1:SCH], scalar1=e256r[:, s_:s_ + 1], scalar2=None, op0=ALU.mult))
                    q2 = K.I("dve", [q1], lambda e: e.scalar_tensor_tensor(out=zin[:, np_, 0, s_:s_ + 1], in0=zz[:, k, 1, SCH - 1:SCH], scalar=e256in[:, s_:s_ + 1], in1=tmp[:, 0, s_:s_ + 1], op0=ALU.mult, op1=ALU.add))
                    q3 = K.I("dve", [q2], lambda e: e.tensor_scalar(out=tmp[:, 1, s_:s_ + 1], in0=zz[:, k, 1, SCH - 1:SCH], scalar1=e256r[:, s_:s_ + 1], scalar2=None, op0=ALU.mult))
                    q4 = K.I("dve", [q3], lambda e: e.scalar_tensor_tensor(out=zin[:, np_, 1, s_:s_ + 1], in0=zz[:, k, 0, SCH - 1:SCH], scalar=e256i[:, s_:s_ + 1], in1=tmp[:, 1, s_:s_ + 1], op0=ALU.mult, op1=ALU.add))
                    zin_tok[np_][s_] = q4
                    lastz = q4
                else:
                    lastz = z2
                if not own:
                    wkfree[k] = [lastz, p1, p2]
                    continue
                r1 = K.I("dve", [lastz], lambda e: e.tensor_tensor(out=w(1), in0=zz[:, k, 0, :], in1=cs, op=ALU.mult))
                r2 = K.I("pool", [z2, p2], lambda e: e.tensor_tensor(out=w(3), in0=zz[:, k, 1, :], in1=sn, op=ALU.mult))
                r3 = K.I("pool", [r1, r2, xbfree], lambda e: e.tensor_tensor(out=xb[:, s_, 0, :], in0=w(1), in1=w(3), op=ALU.subtract))
                r4 = K.I("dve", [], lambda e: e.tensor_tensor(out=w(4), in0=zz[:, k, 1, :], in1=cs, op=ALU.mult))
                r5 = K.I("pool", [], lambda e: e.tensor_tensor(out=w(5), in0=zz[:, k, 0, :], in1=sn, op=ALU.mult))
                r6 = K.I("pool", [r4, r5], lambda e: e.tensor_tensor(out=xb[:, s_, 1, :], in0=w(4), in1=w(5), op=ALU.add))
                wkfree[k] = [r6, lastz]
                t_x.append(r6)
            ufree[ui] = mm2
            if not own:
                continue
            oc = c - NSCH
            fi = oc % 2
            t_uf = K.dma("sp", "b1f%d" % fi, uf[:, fi, :, :], io["uT"][:, c0:c0 + SCH].rearrange("(b p) t -> p b t", p=128), deps=[uffree[fi]])
            g_t = None
            for cb in range(4):
                yb = cb % 2
                mmy = None
                for s4 in range(4):
                    s_ = cb * 4 + s4
                    K.I("pe", [t_x[s_], g_t if s4 == 0 else None, t_prep], lambda e: e.matmul(pyy[:, yb, :], lhsT=cpr[:, s_, :], rhs=xb[:, s_, 0, :], start=(s4 == 0), stop=False))
                    mmy = K.I("pe", [], lambda e: e.matmul(pyy[:, yb, :], lhsT=cpi[:, s_, :], rhs=xb[:, s_, 1, :], start=False, stop=(s4 == 3)))
                y0 = K.I("dve", [mmy, t_uf, gl_tok], lambda e: e.scalar_tensor_tensor(out=ys[:, cb, :], in0=uf[:, fi, cb, :], scalar=dsk[:, cb:cb + 1], in1=pyy[:, yb, :], op0=ALU.mult, op1=ALU.add))
                y1 = K.I("dve", [y0], lambda e: e.tensor_tensor(out=sq[:, cb, :], in0=ys[:, cb, :], in1=ys[:, cb, :], op=ALU.mult))
                y2 = K.I("dve", [y1], lambda e: e.tensor_scalar(out=sq[:, cb, :], in0=sq[:, cb, :], scalar1=0.044715, scalar2=1.0, op0=ALU.mult, op1=ALU.add))
                y3 = K.I("dve", [y2], lambda e: e.tensor_tensor(out=sq[:, cb, :], in0=sq[:, cb, :], in1=ys[:, cb, :], op=ALU.mult))
                y4 = K.I("act", [y3], lambda e: e.activation(out=sq[:, cb, :], in_=sq[:, cb, :], func=AF.Sigmoid, scale=2.0 * float(np.sqrt(2.0 / np.pi))))
                y5 = K.I("dve", [y4], lambda e: e.tensor_tensor(out=gg[:, cb, :], in0=ys[:, cb, :], in1=sq[:, cb, :], op=ALU.mult))
                g_t = K.I("act", [y5], lambda e: e.activation(out=gb[:, cb, :], in_=gg[:, cb, :], func=AF.Copy))
            xbfree = mmy
            uffree[fi] = y0
            t_sq = None
            for ob_ in range(4):
                gi_ = ob_ % 2
                mg = None
                for kc in range(4):
                    mg = K.I("pe", [g_t, tw, t_sq if kc == 0 else None], lambda e: e.matmul(pgl[:, gi_, :], lhsT=wgl[:, kc, ob_ * 128:(ob_ + 1) * 128], rhs=gb[:, kc, :], start=(kc == 0), stop=(kc == 3)))
                s1 = K.I("act", [mg], lambda e: e.activation(out=sq[:, ob_, :], in_=pgl[:, gi_, :], func=AF.Sigmoid, bias=bgl[:, ob_:ob_ + 1], scale=1.0))
                s2_ = K.I("dve", [s1], lambda e: e.tensor_tensor(out=ys[:, ob_, :], in0=gg[:, ob_, :], in1=sq[:, ob_, :], op=ALU.mult))
                t_sq = K.I("dve", [s2_], lambda e: e.tensor_tensor(out=sq[:, ob_, :], in0=ys[:, ob_, :], in1=ys[:, ob_, :], op=ALU.mult))
            mss = None
            for ob_ in range(4):
                mss = K.I("pe", [t_sq, C["tok"]], lambda e: e.matmul(pss[:, :], lhsT=C["onesf"][:], rhs=sq[:, ob_, :], start=(ob_ == 0), stop=(ob_ == 3)))
            n1 = K.I("act", [mss], lambda e: e.activation(out=rs[:], in_=pss[:, :], func=AF.Sqrt, bias=C["eps_rms"][:, 0:1], scale=1.0 / 512.0))
            n2 = K.I("dve", [n1], lambda e: e.reciprocal(out=rs[:], in_=rs[:]))
            n3 = None
            for ob_ in range(4):
                n3 = K.I("dve", [n2, yofree[fi]], lambda e: e.scalar_tensor_tensor(out=yo[:, fi, ob_, :], in0=ys[:, ob_, :], scalar=gn[:, ob_:ob_ + 1], in1=rs[:], op0=ALU.mult, op1=ALU.mult))
            gl_tok = n3
            yofree[fi] = K.dma("sp", "b1o%d" % fi, io["ycat"][0:512, c0:c0 + SCH].rearrange("(b p) t -> p b t", p=128), yo[:, fi, :, :], deps=[n3])
    K.phase_end()


def rms_rstd(K, C, pss, rs, n, deps):
    n1 = K.I("act", deps, lambda e: e.activation(out=rs, in_=pss, func=AF.Sqrt, bias=C["eps_rms"][:, 0:1], scale=1.0 / n))
    return K.I("dve", [n1], lambda e: e.reciprocal(out=rs, in_=rs))


def stage_B2(K, C, io):
    nc = K.nc
    with ExitStack() as es:
        sb = lambda n, shp, d: es.enter_context(nc.sbuf_tensor(n, shp, d))
        y = sb("c_y", [128, 2, 4, TB], F32); sq = sb("c_sq", [128, 4, TB], F32); rs = sb("c_rs", [128, TB], F32)
        yo = sb("c_yo", [128, 2, 4, TB], BF16)
        zp = sb("c_zp", [128, 4, 2], F32); cbf = sb("c_cbf", [128, 4, 2], F32); cw = sb("c_cw", [128, 4, 3], F32)
        gn = sb("c_gn", [128, 16], F32); fx = sb("c_fx", [128, 4, 4], F32)
        pss = es.enter_context(nc.psum_tensor("c_pss", [128, TB], F32))
        K.dma("sp", "b2p", zp[:], io["zlast_prev"].rearrange("(b p) t -> p b t", p=128))
        K.dma("sp", "b2p", cbf[:], io["cbf"].rearrange("(b p) t -> p b t", p=128))
        K.dma("sp", "b2p", cw[:], io["conv_w"][:, :, :])
        tp = K.dma("sp", "b2p", gn[:], io["gnorm"][:, :])
        f = K.I("dve", [tp], lambda e: e.tensor_tensor(out=fx[:, :, 0:1], in0=zp[:, :, 0:1], in1=cw[:, :, 0:1], op=ALU.mult))
        f = K.I("dve", [f], lambda e: e.tensor_tensor(out=fx[:, :, 1:2], in0=zp[:, :, 1:2], in1=cw[:, :, 1:2], op=ALU.mult))
        f = K.I("dve", [f], lambda e: e.tensor_tensor(out=fx[:, :, 0:1], in0=fx[:, :, 0:1], in1=fx[:, :, 1:2], op=ALU.add))
        f = K.I("dve", [f], lambda e: e.tensor_tensor(out=fx[:, :, 1:2], in0=zp[:, :, 1:2], in1=cw[:, :, 0:1], op=ALU.mult))
        f = K.I("dve", [f], lambda e: e.tensor_tensor(out=fx[:, :, 0:2], in0=fx[:, :, 0:2], in1=cbf[:, :, 0:2], op=ALU.mult))
        yfree = [None, None]
        ofree = [None, None]
        for tb in range(NTB):
            i = tb % 2
            ty = K.dma("sp", "b2y%d" % i, y[:, i, :, :], io["ycT"][:, tb * TB:(tb + 1) * TB].rearrange("(b p) t -> p b t", p=128), deps=[yfree[i]])
            if tb == 0:
                ty = K.I("dve", [ty, f], lambda e: e.tensor_tensor(out=y[:, i, :, 0:2], in0=y[:, i, :, 0:2], in1=fx[:, :, 0:2], op=ALU.add))
            t_sq = K.I("dve", [ty], lambda e: e.tensor_tensor(out=sq[:], in0=y[:, i, :, :], in1=y[:, i, :, :], op=ALU.mult))
            mss = None
            for j in range(4):
                mss = K.I("pe", [t_sq, C["tok"]], lambda e: e.matmul(pss[:, :], lhsT=C["onesf"][:], rhs=sq[:, j, :], start=(j == 0), stop=(j == 3)))
            n2 = rms_rstd(K, C, pss[:, :], rs[:], 512.0, [mss])
            n3 = None
            for j in range(4):
                n3 = K.I("dve", [n2, ofree[i]], lambda e: e.scalar_tensor_tensor(out=yo[:, i, j, :], in0=y[:, i, j, :], scalar=gn[:, 4 + j:5 + j], in1=rs[:], op0=ALU.mult, op1=ALU.mult))
            yfree[i] = n3
            ofree[i] = K.dma("sp", "b2o%d" % i, io["ycat"][512:1024, tb * TB:(tb + 1) * TB].rearrange("(b p) t -> p b t", p=128), yo[:, i, :, :], deps=[n3])
    K.phase_end()


def stage_B3(K, C, io):
    nc = K.nc
    NQ = T // 128
    with ExitStack() as es:
        sb = lambda n, shp, d: es.enter_context(nc.sbuf_tensor(n, shp, d))
        ki2 = sb("i_ki", [128, 2 * T], BF16); qi = sb("i_qi", [128, 8, T], BF16)
        wi = sb("i_wi", [128, NQ, 16], F32); kbr = sb("i_kb", [1, 2 * T], BF16)
        dg = sb("i_dg", [128, 2, 16, 128], BF16); R = sb("i_R", [128, 4, 512], BF16)
        sc = sb("i_sc", [128, 2, 2 * T], F32); junk = sb("i_jk", [128, 2 * T], BF16)
        MB = sb("i_MB", [128, 2 * T], BF16); MT = sb("i_MT", [128, 2, 32, 128], BF16)
        st = sb("i_st", [128, 2, 8], F32)
        pl = es.enter_context(nc.psum_tensor("i_pl", [128, 4, 512], F32))
        psc = es.enter_context(nc.psum_tensor("i_psc", [128, 2, 512], F32))
        ptm = es.enter_context(nc.psum_tensor("i_ptm", [128, 2, 8, 128], BF16))
        for r in range(2):
            K.dma("sp", "b3k", ki2[r * 64:(r + 1) * 64, 0:T], io["kiT_prev"][:, :])
            K.dma("act", "b3k", ki2[r * 64:(r + 1) * 64, T:2 * T], io["kiT"][:, :])
        K.dma("sp", "b3k", qi[:], io["qiT"].rearrange("(b p) t -> p b t", p=128))
        K.dma("act", "b3k", wi[:], io["witok"].rearrange("(q p) h -> p q h", p=128))
        tl = K.dma("act", "b3k", kbr[:], io["keybias"][:, :])
        lfree = [None] * 4
        rfree = [None] * 4
        scfree = [None, None]
        pscfree = [None, None]
        dgfree = [None, None]
        mtfree = [None, None]
        ptmfree = [None, None]
        mb_rd = None
        lc = 0
        kbc = 0
        tcn = 0
        for qt in range(NQ):
            nkt = NQ + 1 + qt
            nk = nkt * 128
            nkb = (nk + 511) // 512
            si = qt % 2
            t_dg = None
            for h in range(16):
                t_dg = K.I("pool", [tl, dgfree[si], C["tok"]], lambda e: e.tensor_scalar(out=dg[:, si, h, :], in0=C["idb"][:], scalar1=wi[:, qt, h:h + 1], scalar2=None, op0=ALU.mult))
            t_ev = None
            for kb in range(nkb):
                ncol = min(512, nk - kb * 512)
                pb = kbc % 2
                kbc += 1
                mdg = None
                for h in range(16):
                    b = lc % 4
                    lc += 1
                    r0 = (h % 2) * 64
                    ml = K.I("pe", [tl, lfree[b]], lambda e: e.matmul(pl[:, b, 0:ncol], lhsT=qi[r0:r0 + 64, h // 2, qt * 128:(qt + 1) * 128], rhs=ki2[r0:r0 + 64, kb * 512:kb * 512 + ncol], start=True, stop=True))
                    if h % 2 == 0:
                        rl = K.I("act", [ml, rfree[b]], lambda e: e.activation(out=R[:, b, 0:ncol], in_=pl[:, b, 0:ncol], func=AF.Relu))
                    else:
                        rl = K.I("dve", [ml, rfree[b]], lambda e: e.tensor_scalar(out=R[:, b, 0:ncol], in0=pl[:, b, 0:ncol], scalar1=0.0, scalar2=None, op0=ALU.max))
                    lfree[b] = rl
                    mdg = K.I("pe", [rl, t_dg, pscfree[pb] if h == 0 else None], lambda e: e.matmul(psc[:, pb, 0:ncol], lhsT=dg[:, si, h, :], rhs=R[:, b, 0:ncol], start=(h == 0), stop=False))
                    rfree[b] = mdg
                last = (kb == nkb - 1)
                mdg = K.I("pe", [C["tok"]], lambda e: e.matmul(psc[:, pb, 0:ncol], lhsT=C["onesb"][0:1, :], rhs=kbr[0:1, kb * 512:kb * 512 + ncol], start=False, stop=not last))
                if last:
                    mdg = K.I("pe", [], lambda e: e.matmul(psc[:, pb, ncol - 128:ncol], lhsT=C["idb"][:], rhs=C["cmask"][:], start=False, stop=True))
                if kb % 2 == 0:
                    t_ev = K.I("act", [mdg, scfree[si]], lambda e: e.activation(out=sc[:, si, kb * 512:kb * 512 + ncol], in_=psc[:, pb, 0:ncol], func=AF.Copy))
                else:
                    t_ev = K.I("dve", [mdg, scfree[si]], lambda e: e.tensor_copy(out=sc[:, si, kb * 512:kb * 512 + ncol], in_=psc[:, pb, 0:ncol]))
                pscfree[pb] = t_ev
                t_evp = t_ev if kb == 0 else [t_evp, t_ev]
            dgfree[si] = mdg
            S = sc[:, si, 0:nk]
            c_ = lambda k: st[:, si, k:k + 1]
            d = K.I("dve", [t_evp], lambda e: e.tensor_reduce(out=c_(0), in_=S, axis=AX.X, op=ALU.max))
            d = K.I("dve", [d], lambda e: e.tensor_reduce(out=c_(1), in_=S, axis=AX.X, op=ALU.min))
            d = K.I("dve", [d], lambda e: e.tensor_scalar(out=c_(2), in0=c_(1), scalar1=-64.0, scalar2=None, op0=ALU.max))
            d = K.I("dve", [d], lambda e: e.tensor_tensor(out=c_(3), in0=c_(0), in1=c_(2), op=ALU.subtract))
            d = K.I("dve", [d], lambda e: e.tensor_scalar(out=c_(3), in0=c_(3), scalar1=0.5, scalar2=None, op0=ALU.mult))
            for it in range(NIT):
                d = K.I("dve", [d], lambda e: e.tensor_tensor(out=c_(4), in0=c_(2), in1=c_(3), op=ALU.add))
                d = K.I("dve", [d, mb_rd], lambda e: e.tensor_scalar(out=junk[:, 0:nk], in0=S, scalar1=c_(4), scalar2=None, op0=ALU.is_gt, op1=ALU.add, accum_out=c_(5)))
                d = K.I("dve", [d], lambda e: e.tensor_scalar(out=c_(6), in0=c_(5), scalar1=TOPK - 0.5, scalar2=None, op0=ALU.is_gt))
                d = K.I("dve", [d], lambda e: e.scalar_tensor_tensor(out=c_(2), in0=c_(6), scalar=c_(3), in1=c_(2), op0=ALU.mult, op1=ALU.add))
                d = K.I("dve", [d], lambda e: e.tensor_scalar(out=c_(3), in0=c_(3), scalar1=0.5, scalar2=None, op0=ALU.mult))
            t_mb = K.I("dve", [d, mb_rd], lambda e: e.tensor_scalar(out=MB[:, 0:nk], in0=S, scalar1=c_(2), scalar2=NEG, op0=ALU.is_le, op1=ALU.mult))
            scfree[si] = t_mb
            t_cp = None
            for kt in range(nkt):
                g4, j4 = kt // 8, kt % 8
                pi_ = g4 % 2
                t_tr = K.I("pe", [t_mb, ptmfree[pi_] if j4 == 0 else None, C["tok"]], lambda e: e.transpose(ptm[:, pi_, j4, :], MB[:, kt * 128:(kt + 1) * 128], C["idb"][:]))
                if j4 == 7 or kt == nkt - 1:
                    n4 = j4 + 1
                    t_cp = K.I("dve", [t_tr, mtfree[si]], lambda e: e.tensor_copy(out=MT[:, si, g4 * 8:g4 * 8 + n4, :], in_=ptm[:, pi_, 0:n4, :]))
                    ptmfree[pi_] = t_cp
                    t_cpa = t_cp if g4 == 0 else [t_cpa, t_cp]
            mb_rd = t_tr
            mtfree[si] = K.dma("sp", "b3o%d" % si, io["mbt"][qt, :, 0:nkt, :], MT[:, si, 0:nkt, :], deps=[t_cpa])
            K.wait("sp", mtfree[si])
            nc.all_engine_barrier()
    K.phase_end()


def stage_B4(K, C, io):
    nc = K.nc
    NQ = T // 128
    with ExitStack() as es:
        sb = lambda n, shp, d: es.enter_context(nc.sbuf_tensor(n, shp, d))
        Ks = sb("t_K", [128, 4, 2 * T], BF16); Vs = sb("t_V", [128, 2 * NQ, 4, 132], BF16)
        Qs = sb("t_Q", [128, 4, T], BF16); Ms = sb("t_M", [128, 2, 32, 128], BF16)
        Pb = sb("t_P", [128, 4, 512], BF16); yst = sb("t_y", [128, 2, 4, 128], F32); rd = sb("t_rd", [128, 8], F32)
        spp = es.enter_context(nc.psum_tensor("t_sp", [128, 4, 512], F32))
        opp = es.enter_context(nc.psum_tensor("t_op", [128, 2, 512], F32))
        t_one = K.I("pool", (), lambda e: e.memset(Vs[:], 1.0))
        kfree = None
        mfree = [None, None]
        yfree = [None, None]
        sfree = [None] * 4
        pfree = [None] * 4
        ofree = [None, None]
        sc_ = 0
        oc_ = 0
        mload = 0
        for hg in range(2):
            tk = None
            for hh in range(4):
                hd = hg * 4 + hh
                K.dma("sp", "b4k", Ks[:, hh, 0:T], io["kT_prev"][hd * 128:(hd + 1) * 128, :], deps=[kfree])
                K.dma("act", "b4k", Ks[:, hh, T:2 * T], io["kT"][hd * 128:(hd + 1) * 128, :], deps=[kfree])
                K.dma("sp", "b4k", Vs[:, 0:NQ, hh, 0:128], io["vtok_prev"][:, hd * 128:(hd + 1) * 128].rearrange("(k p) d -> p k d", p=128), deps=[kfree, t_one])
                K.dma("act", "b4k", Vs[:, NQ:2 * NQ, hh, 0:128], io["vtok"][:, hd * 128:(hd + 1) * 128].rearrange("(k p) d -> p k d", p=128), deps=[kfree, t_one])
                tk = K.dma("sp", "b4k", Qs[:, hh, :], io["qT"][hd * 128:(hd + 1) * 128, :], deps=[kfree])
            for qt in range(NQ):
                nkt = NQ + 1 + qt
                mi = mload % 2
                mload += 1
                tm = K.dma("act", "b4m%d" % mi, Ms[:, mi, 0:nkt, :], io["mbt"][qt, :, 0:nkt, :], deps=[mfree[mi]])
                yi = qt % 2
                lastpv = None
                for hh in range(4):
                    ob_ = oc_ % 2
                    oc_ += 1
                    ngrp = (nkt + 3) // 4
                    pv = None
                    for g in range(ngrp):
                        k0 = g * 4
                        n4 = min(4, nkt - k0)
                        b = sc_ % 4
                        sc_ += 1
                        ms = None
                        for j in range(n4):
                            kt = k0 + j
                            K.I("pe", [tk, tm, sfree[b] if j == 0 else None, C["tok"]], lambda e: e.matmul(spp[:, b, j * 128:(j + 1) * 128], lhsT=Ks[:, hh, kt * 128:(kt + 1) * 128], rhs=Qs[:, hh, qt * 128:(qt + 1) * 128], start=True, stop=False))
                            ms = K.I("pe", [], lambda e: e.matmul(spp[:, b, j * 128:(j + 1) * 128], lhsT=C["idb"][:], rhs=Ms[:, mi, kt, :], start=False, stop=True))
                        ex = K.I("act", [ms, pfree[b]], lambda e: e.activation(out=Pb[:, b, 0:n4 * 128], in_=spp[:, b, 0:n4 * 128], func=AF.Exp))
                        sfree[b] = ex
                        for j in range(n4):
                            kt = k0 + j
                            pv = K.I("pe", [ex, ofree[ob_] if kt == 0 else None], lambda e: e.matmul(opp[:, ob_, 0:129], lhsT=Pb[:, b, j * 128:(j + 1) * 128], rhs=Vs[:, kt, hh, 0:129], start=(kt == 0), stop=(kt == nkt - 1)))
                        pfree[b] = pv
                    lastpv = pv
                    r1 = K.I("dve", [pv], lambda e: e.reciprocal(out=rd[:, hh:hh + 1], in_=opp[:, ob_, 128:129]))
                    r2 = K.I("dve", [r1, yfree[yi] if hh == 0 else None], lambda e: e.tensor_scalar(out=yst[:, yi, hh, :], in0=opp[:, ob_, 0:128], scalar1=rd[:, hh:hh + 1], scalar2=None, op0=ALU.mult))
                    ofree[ob_] = r2
                mfree[mi] = lastpv
                yfree[yi] = K.dma("sp", "b4o%d" % yi, io["yattn"][qt * 128:(qt + 1) * 128, hg * 512:(hg + 1) * 512].rearrange("q (h d) -> q h d", h=4), yst[:, yi, :, :], deps=[r2])
            kfree = lastpv
    K.phase_end()


def stage_B5(K, C, io):
    nc = K.nc
    NQ = T // 128
    with ExitStack() as es:
        sb = lambda n, shp, d: es.enter_context(nc.sbuf_tensor(n, shp, d))
        y = sb("p_y", [128, 2, 1024], F32); jk = sb("p_jk", [128, 1024], F32); yb = sb("p_yb", [128, 2, 1024], BF16)
        ss = sb("p_ss", [128, 2, 2], F32); gn = sb("p_gn", [128, 16], F32); yo = sb("p_yo", [128, 2, 8, 128], BF16)
        ptp = es.enter_context(nc.psum_tensor("p_pt", [128, 2, 8, 128], BF16))
        tg = K.dma("sp", "b5g", gn[:], io["gnorm"][:, :])
        yfree = [None, None]
        ofree = [None, None]
        pfree = [None, None]
        for qt in range(NQ):
            i = qt % 2
            ty = K.dma("sp" if i == 0 else "act", "b5y%d" % i, y[:, i, :], io["yattn"][qt * 128:(qt + 1) * 128, :], deps=[yfree[i]])
            a = K.I("act", [ty], lambda e: e.activation(out=jk[:], in_=y[:, i, :], func=AF.Square, accum_out=ss[:, i, 0:1]))
            a = K.I("act", [a], lambda e: e.activation(out=ss[:, i, 1:2], in_=ss[:, i, 0:1], func=AF.Sqrt, bias=C["eps_rms"][:, 0:1], scale=1.0 / 1024.0))
            a = K.I("dve", [a], lambda e: e.reciprocal(out=ss[:, i, 1:2], in_=ss[:, i, 1:2]))
            a = K.I("dve", [a, pfree[i]], lambda e: e.tensor_scalar(out=yb[:, i, :], in0=y[:, i, :], scalar1=ss[:, i, 1:2], scalar2=None, op0=ALU.mult))
            yfree[i] = a
            tt = None
            for hb in range(8):
                tt = K.I("pe", [a, ofree[i] if hb == 0 else None, C["tok"]], lambda e: e.transpose(ptp[:, i, hb, :], yb[:, i, hb * 128:(hb + 1) * 128], C["idb"][:]))
            pfree[i] = tt
            cpt = None
            for hb in range(8):
                cpt = K.I("dve", [tt, tg], lambda e: e.tensor_scalar(out=yo[:, i, hb, :], in0=ptp[:, i, hb, :], scalar1=gn[:, 8 + hb:9 + hb], scalar2=None, op0=ALU.mult))
                cpa = cpt if hb == 0 else [cpa, cpt]
            ofree[i] = K.dma("sp", "b5o%d" % i, io["ycat"][1024:2048, qt * 128:(qt + 1) * 128].rearrange("(b p) t -> p b t", p=128), yo[:, i, :, :], deps=[cpa])
            ofree[i] = [ofree[i], cpa]
    K.phase_end()


def ln_block(K, C, r, t_r, tb, tiles, gam, bet, outs, keyp):
    sq, mean, rstd, msq, xo, ho, ps1, ps2 = (tiles[k] for k in ("sq", "mean", "rstd", "msq", "xo", "ho", "ps1", "ps2"))
    x_dram, h_dram, hs, hb = outs
    m1 = None
    m2 = None
    tq = None
    for db in range(KC):
        m1 = K.I("pe", [t_r, tiles.get("psfree"), C["tok"]], lambda e: e.matmul(ps1, lhsT=C["onesf"][:], rhs=r[:, db, :], start=(db == 0), stop=(db == KC - 1)))
    for db in range(KC):
        si = db % 2
        tq = K.I("act", [t_r, tiles["sqfree"][si]], lambda e: e.activation(out=sq[:, si, :], in_=r[:, db, :], func=AF.Square))
        m2 = K.I("pe", [tq], lambda e: e.matmul(ps2, lhsT=C["onesf"][:], rhs=sq[:, si, :], start=(db == 0), stop=(db == KC - 1)))
        tiles["sqfree"][si] = m2
    a = K.I("dve", [m1, tiles.get("stfree")], lambda e: e.tensor_scalar(out=mean, in0=ps1, scalar1=1.0 / D, scalar2=None, op0=ALU.mult))
    a = K.I("dve", [a], lambda e: e.tensor_tensor(out=msq, in0=mean, in1=mean, op=ALU.mult))
    a = K.I("dve", [a, m2], lambda e: e.scalar_tensor_tensor(out=rstd, in0=ps2, scalar=1.0 / D, in1=msq, op0=ALU.mult, op1=ALU.subtract))
    tiles["psfree"] = a
    a = K.I("act", [a], lambda e: e.activation(out=rstd, in_=rstd, func=AF.Sqrt, bias=C["eps_ln"][:, 0:1], scale=1.0))
    a = K.I("dve", [a], lambda e: e.reciprocal(out=rstd, in_=rstd))
    last = None
    for db in range(KC):
        oi = db % 2
        b1 = K.I("dve", [a], lambda e: e.tensor_tensor(out=r[:, db, :], in0=r[:, db, :], in1=mean, op=ALU.subtract))
        b2 = K.I("dve", [b1], lambda e: e.tensor_tensor(out=r[:, db, :], in0=r[:, db, :], in1=rstd, op=ALU.mult))
        b3 = K.I("act", [b2, tiles["xofree"][oi]], lambda e: e.activation(out=xo[:, oi, :], in_=r[:, db, :], func=AF.Identity, bias=bet[:, db:db + 1], scale=gam[:, db:db + 1]))
        d1 = K.dma("sp", keyp + "lx%d" % oi, x_dram[db * 128:(db + 1) * 128, tb * TB:(tb + 1) * TB], xo[:, oi, :], deps=[b3])
        tiles["xofree"][oi] = d1
        last = [d1]
        if h_dram is not None:
            b4 = K.I("act", [b3, tiles["hofree"][oi]], lambda e: e.activation(out=ho[:, oi, :], in_=xo[:, oi, :], func=AF.Identity, bias=hb[:, db:db + 1], scale=hs[:, db:db + 1]))
            d2 = K.dma("act", keyp + "lh%d" % oi, h_dram[db * 128:(db + 1) * 128, tb * TB:(tb + 1) * TB], ho[:, oi, :], deps=[b4])
            tiles["hofree"][oi] = d2
            tiles["xofree"][oi] = [d1, b4]
            last = [d1, d2]
    tiles["stfree"] = b2
    return last, [b2, b3]


def ln_tiles(nc, es, pfx, with_h):
    sb = lambda n, shp, d: es.enter_context(nc.sbuf_tensor(pfx + n, shp, d))
    t = dict(sq=sb("sq", [128, 2, TB], F32), mean=sb("mean", [128, TB], F32)[:], rstd=sb("rstd", [128, TB], F32)[:],
             msq=sb("msq", [128, TB], F32)[:], xo=sb("xo", [128, 2, TB], F32), ho=sb("ho", [128, 2, TB], BF16) if with_h else None)
    pp = es.enter_context(nc.psum_tensor(pfx + "pln", [128, 2, TB], F32))
    t["ps1"] = pp[:, 0, :]
    t["ps2"] = pp[:, 1, :]
    t["sqfree"] = [None, None]; t["xofree"] = [None, None]; t["hofree"] = [None, None]
    return t


def stage_B6(K, C, l, io):
    nc = K.nc
    with ExitStack() as es:
        sb = lambda n, shp, d: es.enter_context(nc.sbuf_tensor(n, shp, d))
        yc = sb("o_yc", [128, KC, T], BF16); mod = sb("o_mod", [128, 96], F32)
        g1 = sb("o_g1", [128, KC], F32); sc2 = sb("o_sc2", [128, KC], F32)
        gam = sb("o_gam", [128, KC], F32); bet = sb("o_bet", [128, KC], F32)
        wsb = sb("o_w", [128, 4, KC, 128], BF16); xin = sb("o_x", [128, 4, TB], F32); r = sb("o_r", [128, KC, TB], F32)
        ps = es.enter_context(nc.psum_tensor("o_ps", [128, 4, TB], F32))
        lt = ln_tiles(nc, es, "o_", True)
        K.dma("sp", "b6p", mod[:], io["modT"][:, :]); K.dma("sp", "b6p", gam[:], io["ln1_g"][:, :])
        tp = K.dma("sp", "b6p", bet[:], io["ln1_b"][:, :])
        ty = K.dma("sp", "b6y", yc[:, 0:8, :], io["ycat"][0:1024, :].rearrange("(b p) t -> p b t", p=128))
        ty = K.dma("act", "b6y", yc[:, 8:16, :], io["ycat"][1024:2048, :].rearrange("(b p) t -> p b t", p=128))
        a = K.I("dve", [tp], lambda e: e.tensor_scalar(out=g1[:], in0=mod[:, 32:48], scalar1=1.0 / ALPHA, scalar2=None, op0=ALU.mult))
        tpp = K.I("dve", [a], lambda e: e.tensor_scalar(out=sc2[:], in0=mod[:, 64:80], scalar1=1.0, scalar2=None, op0=ALU.add))
        wfree = [None] * 4
        psfree = [None] * 4
        xfree = [None] * 4
        n = 0
        t_ln = None
        for tb in range(NTB):
            t_r = None
            for db in range(KC):
                s = n % 4
                n += 1
                tw = K.dma("pool", "b6w%d" % s, wsb[:, s, :, :], io["w_o"][:, :, db * 128:(db + 1) * 128], deps=[wfree[s]])
                tx = K.dma("sp", "b6x%d" % s, xin[:, s, :], io["xT"][db * 128:(db + 1) * 128, tb * TB:(tb + 1) * TB], deps=[xfree[s]])
                mm = None
                for kc in range(KC):
                    mm = K.I("pe", [tw, ty, psfree[s]], lambda e: e.matmul(ps[:, s, :], lhsT=wsb[:, s, kc, :], rhs=yc[:, kc, tb * TB:(tb + 1) * TB], start=(kc == 0), stop=(kc == KC - 1)))
                wfree[s] = mm
                t_r = K.I("dve", [mm, tx, tpp, t_ln], lambda e: e.scalar_tensor_tensor(out=r[:, db, :], in0=ps[:, s, :], scalar=g1[:, db:db + 1], in1=xin[:, s, :], op0=ALU.mult, op1=ALU.add))
                psfree[s] = t_r
                xfree[s] = t_r
            _, t_ln = ln_block(K, C, r, t_r, tb, lt, gam, bet, (io["x1T"], io["h2T"], sc2, mod[:, 48:64]), "b6")
    K.phase_end()


def stage_B7(K, C, l, io):
    nc = K.nc
    NF = D_FF // 128
    with ExitStack() as es:
        sb = lambda n, shp, d: es.enter_context(nc.sbuf_tensor(n, shp, d))
        h2 = sb("f_h2", [128, 2, KC, TB], BF16) if False else sb("f_h2", [128, KC, TB], BF16)
        hid = sb("f_hid", [128, NF, TB], BF16); mod = sb("f_mod", [128, 96], F32); g2 = sb("f_g2", [128, KC], F32)
        gam = sb("f_gam", [128, KC], F32); bet = sb("f_bet", [128, KC], F32)
        w1 = sb("f_w1", [128, 4, KC, 128], BF16); w2 = sb("f_w2", [128, 2, NF, 128], BF16)
        r1 = sb("f_r1", [128, 2, TB], BF16); xin = sb("f_x", [128, 2, TB], F32); r = sb("f_r", [128, KC, TB], F32)
        ps = es.enter_context(nc.psum_tensor("f_ps", [128, 4, TB], F32))
        lt = ln_tiles(nc, es, "f_", False)
        K.dma("sp", "b7p", mod[:], io["modT"][:, :]); K.dma("sp", "b7p", gam[:], io["ln2_g"][:, :])
        tp = K.dma("sp", "b7p", bet[:], io["ln2_b"][:, :])
        tpp = K.I("dve", [tp], lambda e: e.tensor_scalar(out=g2[:], in0=mod[:, 80:96], scalar1=1.0 / ALPHA, scalar2=None, op0=ALU.mult))
        w1free = [None] * 4
        w2free = [None] * 2
        psfree = [None] * 4
        r1free = [None, None]
        xfree = [None, None]
        hfree = None
        hidfree = None
        t_ln = None
        n1 = 0
        n2 = 0
        pc = 0
        for tb in range(NTB):
            th = K.dma("sp", "b7h", h2[:], io["h2T"][:, tb * TB:(tb + 1) * TB].rearrange("(b p) t -> p b t", p=128), deps=[hfree])
            t_hid = None
            for fb in range(NF):
                s = n1 % 4
                n1 += 1
                b = pc % 4
                pc += 1
                tw = K.dma("pool", "b7w%d" % s, w1[:, s, :, :], io["w_ff1"][:, :, fb * 128:(fb + 1) * 128], deps=[w1free[s]])
                mm = None
                for kc in range(KC):
                    mm = K.I("pe", [tw, th, psfree[b]], lambda e: e.matmul(ps[:, b, :], lhsT=w1[:, s, kc, :], rhs=h2[:, kc, :], start=(kc == 0), stop=(kc == KC - 1)))
                w1free[s] = mm
                ri = fb % 2
                a1 = K.I("act", [mm, r1free[ri]], lambda e: e.activation(out=r1[:, ri, :], in_=ps[:, b, :], func=AF.Relu))
                t_hid = K.I("dve", [a1, hidfree], lambda e: e.scalar_tensor_tensor(out=hid[:, fb, :], in0=ps[:, b, :], scalar=0.0, in1=r1[:, ri, :], op0=ALU.max, op1=ALU.mult))
                psfree[b] = t_hid
                r1free[ri] = t_hid
            hfree = mm
            t_r = None
            for db in range(KC):
                s = n2 % 2
                n2 += 1
                b = pc % 4
                pc += 1
                tw = K.dma("pool", "b7v%d" % s, w2[:, s, :, :], io["w_ff2"][:, :, db * 128:(db + 1) * 128], deps=[w2free[s]])
                tx = K.dma("sp", "b7x%d" % s, xin[:, s, :], io["x1T"][db * 128:(db + 1) * 128, tb * TB:(tb + 1) * TB], deps=[xfree[s]])
                mm = None
                for fc in range(NF):
                    mm = K.I("pe", [tw, t_hid, psfree[b]], lambda e: e.matmul(ps[:, b, :], lhsT=w2[:, s, fc, :], rhs=hid[:, fc, :], start=(fc == 0), stop=(fc == NF - 1)))
                w2free[s] = mm
                t_r = K.I("dve", [mm, tx, tpp, t_ln], lambda e: e.scalar_tensor_tensor(out=r[:, db, :], in0=ps[:, b, :], scalar=g2[:, db:db + 1], in1=xin[:, s, :], op0=ALU.mult, op1=ALU.add))
                psfree[b] = t_r
                xfree[s] = t_r
            hidfree = mm
            _, t_ln = ln_block(K, C, r, t_r, tb, lt, gam, bet, (io["x2T"], None, None, None), "b7")
    K.phase_end()

A_OUT = dict(modT=([128, 96], F32), uT=([512, T], F32), ycT=([512, T], F32), cbf=([512, 2], F32), zlast=([512, 2], F32),
             qT=([1024, T], BF16), kT=([1024, T], BF16), vtok=([T, 1024], BF16), qiT=([1024, T], BF16),
             kiT=([64, T], BF16), witok=([T, 16], F32))
A_IN = dict(xT=([D, T], F32), cvec=([128, KC], F32), b_ada=([128, 96], F32), w_ada=([128, KC, 6 * D], F32),
            w_in=([128, KC, D_IN], F32), conv_w=([128, 4, 3], F32),
            cosA=([128, T], F32), sinA=([128, T], F32), cosI=([128, T], F32), sinI=([128, T], F32))


B_IN_OWN = dict(modT=([128, 96], F32), uT=([512, T], F32), ycT=([512, T], F32), cbf=([512, 2], F32),
                qT=([1024, T], BF16), kT=([1024, T], BF16), vtok=([T, 1024], BF16), qiT=([1024, T], BF16),
                kiT=([64, T], BF16), witok=([T, 16], F32))
B_IN_PREV = dict(kT_prev=([1024, T], BF16), vtok_prev=([T, 1024], BF16), kiT_prev=([64, T], BF16),
                 uT_prev=([512, T], F32), zlast_prev=([512, 2], F32))
B_IN_W = dict(xT=([D, T], F32), keybias=([1, 2 * T], BF16), lam_re=([128, 16], F32), lam_im=([128, 16], F32), log_dt=([128, 16], F32),
              ssm_b_re=([128, 16, 16], F32), ssm_b_im=([128, 16, 16], F32), ssm_c_re=([128, 16, 16], F32),
              ssm_c_im=([128, 16, 16], F32), ssm_d=([128, 4], F32), w_glu=([128, 4, 512], F32), b_glu=([128, 4], F32),
              gnorm=([128, 16], F32), conv_w=([128, 4, 3], F32), w_o=([128, KC, D], F32), ln1_g=([128, KC], F32),
              ln1_b=([128, KC], F32), w_ff1=([128, KC, D_FF], F32), w_ff2=([128, D_FF // 128, D], F32),
              ln2_g=([128, KC], F32), ln2_b=([128, KC], F32))
B_INTERNAL = dict(ycat=([D, T], BF16), mbt=([T // 128, 128, 32, 128], BF16), yattn=([T, 1024], F32),
                  x1T=([D, T], F32), h2T=([D, T], BF16))


def stage_B(K, C, l, io, only=None):
    stages = dict(b1=lambda: stage_B1(K, C, io), b2=lambda: stage_B2(K, C, io), b3=lambda: stage_B3(K, C, io),
                  b4=lambda: stage_B4(K, C, io), b5=lambda: stage_B5(K, C, io), b6=lambda: stage_B6(K, C, l, io),
                  b7=lambda: stage_B7(K, C, l, io))
    for n, f in stages.items():
        if only is None or n in only:
            f()


def build_B(l, debug=False, only=None):
    K = Prog()
    io = {}
    for dct in (B_IN_OWN, B_IN_PREV, B_IN_W):
        for n, (shp, dt_) in dct.items():
            io[n] = K.dt(n, shp, dt_, "ExternalInput")
    for n, (shp, dt_) in B_INTERNAL.items():
        io[n] = K.dt(n, shp, dt_, "ExternalOutput" if debug else "Internal")
    io["x2T"] = K.dt("x2T", [D, T], F32, "ExternalOutput")
    C = make_consts(K)
    K.phase_end()
    stage_B(K, C, l, io, only)
    return K


def host_B_weights(inp, l):
    g = lambda n: inp[n][l]
    st = lambda a: np.ascontiguousarray(a.reshape(16, 128).T)
    bl = lambda a: np.ascontiguousarray(a.reshape(16, 128, 16).transpose(1, 0, 2))
    return dict(
        lam_re=st(g("lam_re")), lam_im=st(g("lam_im")), log_dt=st(np.repeat(g("log_dt"), 64)),
        ssm_b_re=bl(g("ssm_b_re")), ssm_b_im=bl(g("ssm_b_im")),
        ssm_c_re=bl(g("ssm_c_re").transpose(0, 2, 1)), ssm_c_im=bl(g("ssm_c_im").transpose(0, 2, 1)),
        ssm_d=vec_p(g("ssm_d")), w_glu=tile_k(g("w_glu")), b_glu=vec_p(g("b_glu")), gnorm=vec_p(g("gnorm_g")),
        conv_w=np.ascontiguousarray(g("conv_w").T.reshape(4, 128, 3).transpose(1, 0, 2)),
        w_o=tile_k(g("w_o")), ln1_g=vec_p(g("ln1_g")), ln1_b=vec_p(g("ln1_b")),
        w_ff1=tile_k(g("w_ff1")), w_ff2=tile_k(g("w_ff2")), ln2_g=vec_p(g("ln2_g")), ln2_b=vec_p(g("ln2_b")))


def host_keybias(half):
    kb = np.zeros((1, 2 * T), np.float32)
    if half == 0:
        kb[0, :T] = NEG
    return kb.astype(ml_dtypes.bfloat16)


def host_prev(a_out, core):
    half = core % 2
    src = a_out[core - 1] if half == 1 else None
    d = {}
    for n, (shp, dt_) in B_IN_PREV.items():
        base = n[:-5]
        d[n] = np.ascontiguousarray(src[base]) if src is not None else np.zeros(shp, np_dt(dt_))
    return d


def build_A(l):
    K = Prog()
    io = {}
    for n, (shp, dt_) in A_IN.items():
        io[n] = K.dt(n, shp, dt_, "ExternalInput")
    for n, (shp, dt_) in A_OUT.items():
        io[n] = K.dt(n, shp, dt_, "ExternalOutput")
    C = make_consts(K)
    stage_A(K, C, l, io)
    return K


def rope_tables_np(pos, dim):
    inv = (1.0 / (np.float32(10000.0) ** (np.arange(0, dim, 2, dtype=np.float32) / np.float32(dim)))).astype(np.float32)
    ang = pos.astype(np.float32)[:, None] * inv[None, :]
    return np.cos(ang).astype(np.float32), np.sin(ang).astype(np.float32)


def host_tables(half):
    pos = np.arange(half * T, (half + 1) * T)
    ca, sa = rope_tables_np(pos, 128)
    ci, si = rope_tables_np(pos, 64)
    cosA = np.concatenate([ca.T, ca.T], 0)
    sinA = np.concatenate([-sa.T, sa.T], 0)
    cosI = np.concatenate([ci.T] * 4, 0)
    sinI = np.concatenate([-si.T, si.T, -si.T, si.T], 0)
    return dict(cosA=np.ascontiguousarray(cosA), sinA=np.ascontiguousarray(sinA),
                cosI=np.ascontiguousarray(cosI), sinI=np.ascontiguousarray(sinI))


def tile_k(w):
    k, n = w.shape
    return np.ascontiguousarray(w.reshape(k // 128, 128, n).transpose(1, 0, 2))


def vec_p(v):
    return np.ascontiguousarray(v.reshape(-1, 128).T)


def host_A_inputs(inp, l, xT_cores):
    maps = []
    w_ada = tile_k(inp["w_ada"][l])
    w_in = tile_k(inp["w_in"][l])
    b_ada = vec_p(inp["b_ada"][l])
    conv_w = np.ascontiguousarray(inp["conv_w"][l].T.reshape(4, 128, 3).transpose(1, 0, 2))
    for core in range(8):
        b, half = core // 2, core % 2
        m = dict(xT=xT_cores[core], cvec=vec_p(inp["c"][b]), b_ada=b_ada, w_ada=w_ada, w_in=w_in, conv_w=conv_w)
        m.update(host_tables(half))
        maps.append(m)
    return maps


SFX = "_n"


def build_BA(l):
    K = Prog()
    io = {}
    for dct in (B_IN_OWN, B_IN_PREV, B_IN_W):
        for n, (shp, dt_) in dct.items():
            io[n] = K.dt(n, shp, dt_, "ExternalInput")
    for n, (shp, dt_) in B_INTERNAL.items():
        io[n] = K.dt(n, shp, dt_, "Internal")
    io["x2T"] = K.dt("x2T", [D, T], F32, "ExternalOutput")
    ioa = {}
    for n, (shp, dt_) in A_IN.items():
        if n == "xT":
            ioa[n] = io["x2T"]
        elif n in ("cosA", "sinA", "cosI", "sinI", "cvec"):
            ioa[n] = K.dt(n, shp, dt_, "ExternalInput")
        else:
            ioa[n] = K.dt(n + SFX, shp, dt_, "ExternalInput")
    for n, (shp, dt_) in A_OUT.items():
        ioa[n] = K.dt(n + SFX, shp, dt_, "ExternalOutput")
    C = make_consts(K)
    K.phase_end()
    stage_B(K, C, l, io)
    stage_A(K, C, l + 1, ioa)
    return K


def _run(K, maps):
    res = run_bass_kernel_spmd(K.nc, maps, core_ids=list(range(8)))
    return res.results


def _b_maps(inp, l, a_out, xT_cores):
    W = host_B_weights(inp, l)
    maps = []
    for core in range(8):
        m = {k: np.ascontiguousarray(a_out[core][k]) for k in B_IN_OWN}
        m.update(host_prev(a_out, core))
        m.update(W)
        m["xT"] = xT_cores[core]
        m["keybias"] = host_keybias(core % 2)
        maps.append(m)
    return maps


def kernel(**inp):
    inp = {k: np.asarray(v) for k, v in inp.items()}
    x = inp["x"].astype(np.float32)
    xT_cores = [np.ascontiguousarray(x[c // 2, (c % 2) * T:(c % 2 + 1) * T, :].T) for c in range(8)]
    a0 = _run(build_A(0), host_A_inputs(inp, 0, xT_cores))
    maps = _b_maps(inp, 0, a0, xT_cores)
    a_in1 = host_A_inputs(inp, 1, xT_cores)
    for core in range(8):
        for n in A_IN:
            if n == "xT":
                continue
            key = n if n in ("cosA", "sinA", "cosI", "sinI", "cvec") else n + SFX
            maps[core][key] = a_in1[core][n]
    r2 = _run(build_BA(0), maps)
    x1T_cores = [np.ascontiguousarray(r2[c]["x2T"]) for c in range(8)]
    a1 = [{n: r2[c][n + SFX] for n in A_OUT} for c in range(8)]
    r3 = _run(build_B(1), _b_maps(inp, 1, a1, x1T_cores))
    out = np.empty((4, 2 * T, D), np.float32)
    for c in range(8):
        out[c // 2, (c % 2) * T:(c % 2 + 1) * T, :] = np.asarray(r3[c]["x2T"]).T
    return out
```
